# Optimizing a Trainium2 kernel written in Bass

```python
import jax, jax.numpy as jnp
from jax import lax
import numpy as np

D_MODEL = 1024
BATCH = 32
SEQ = 2048
DEPTH = 2

D_MIX = D_MODEL
HEAD_DIM = 64
A_HEADS = 6
A_DIM = A_HEADS * HEAD_DIM
IDX_HEADS = 4
IDX_DIM = 64
TOPK_MAX = 256
Q_BLOCK = 128
B_HEADS = 6
B_DK = 64
B_DV = 64
B_DIM = B_HEADS * B_DV
HGRN_CHUNK = 64
C_GROUPS = 4
C_DIM = D_MIX - A_DIM - B_DIM
C_GROUP_DIM = C_DIM // C_GROUPS
C_CHUNK = 128
ROPE_THETA = 10000.0
F_DENSE = 2816
N_EXPERTS = 8
TOP_K = 2
F_EXPERT = 3584
N_DENSE = (DEPTH + 1) // 2
N_MOE = DEPTH // 2
EPS = 1e-6
IN_WIDTHS = (A_DIM, HEAD_DIM, HEAD_DIM, IDX_HEADS * IDX_DIM, IDX_DIM, IDX_HEADS, B_HEADS * B_DK, B_HEADS * B_DK, B_HEADS * B_DV, B_HEADS * B_DV, 2 * C_DIM)
IN_DIM = sum(IN_WIDTHS)

kernel_name = "hybrid_dsa_hgrn2_gmlp_moe_block"


def rms_norm(x, g):
    xf = x.astype(jnp.float32)
    y = xf * lax.rsqrt(jnp.mean(xf * xf, axis=-1, keepdims=True) + EPS)
    return (y * g.astype(jnp.float32)).astype(x.dtype)


def layer_norm(x, g, b):
    xf = x.astype(jnp.float32)
    mu = jnp.mean(xf, axis=-1, keepdims=True)
    var = jnp.mean(jnp.square(xf - mu), axis=-1, keepdims=True)
    y = (xf - mu) * lax.rsqrt(var + EPS)
    return (y * g.astype(jnp.float32) + b.astype(jnp.float32)).astype(x.dtype)


def rope(x, pos):
    d = x.shape[-1]
    inv = ROPE_THETA ** (-jnp.arange(0, d, 2, dtype=jnp.float32) / d)
    ang = pos.astype(jnp.float32)[..., None] * inv
    cos = jnp.cos(ang)[:, :, None, :]
    sin = jnp.sin(ang)[:, :, None, :]
    xf = x.astype(jnp.float32)
    x1, x2 = xf[..., : d // 2], xf[..., d // 2:]
    return jnp.concatenate([x1 * cos - x2 * sin, x2 * cos + x1 * sin], axis=-1).astype(x.dtype)


def dsa_mixer(q, k, v, iq, ik, iw, positions):
    Bn, S = q.shape[0], q.shape[1]
    q = rope(q, positions)
    k = rope(k[:, :, None, :], positions)[:, :, 0, :]
    iq = rope(iq, positions)
    ik = rope(ik[:, :, None, :], positions)[:, :, 0, :]
    n_top = min(TOPK_MAX, S // 4)
    nb = S // Q_BLOCK
    to_blocks = lambda a: jnp.swapaxes(a.reshape((Bn, nb, Q_BLOCK) + a.shape[2:]), 0, 1)
    s_idx = jnp.arange(S)

    def one_block(args):
        qj, iqj, iwj, j = args
        t = j * Q_BLOCK + jnp.arange(Q_BLOCK)
        rel = jax.nn.relu(jnp.einsum('bqhd,bsd->bqhs', iqj, ik).astype(jnp.float32) * IDX_DIM ** -0.5)
        score = jnp.einsum('bqh,bqhs->bqs', iwj.astype(jnp.float32) * IDX_HEADS ** -0.5, rel)
        causal = s_idx[None, :] <= t[:, None]
        score = jnp.where(causal[None], score, -jnp.inf)
        _, idx = lax.top_k(score, n_top)
        flat = idx.reshape(Bn, Q_BLOCK * n_top, 1)
        kg = jnp.take_along_axis(k, flat, axis=1).reshape(Bn, Q_BLOCK, n_top, HEAD_DIM)
        vg = jnp.take_along_axis(v, flat, axis=1).reshape(Bn, Q_BLOCK, n_top, HEAD_DIM)
        logits = jnp.einsum('bqhd,bqkd->bqhk', qj, kg).astype(jnp.float32) * HEAD_DIM ** -0.5
        valid = idx <= t[None, :, None]
        logits = jnp.where(valid[:, :, None, :], logits, -jnp.inf)
        p = jax.nn.softmax(logits, axis=-1).astype(v.dtype)
        return jnp.einsum('bqhk,bqkd->bqhd', p, vg)

    out = lax.map(one_block, (to_blocks(q), to_blocks(iq), to_blocks(iw), jnp.arange(nb)))
    return jnp.swapaxes(out, 0, 1).reshape(Bn, S, A_HEADS * HEAD_DIM)


def hgrn2_mixer(q, f_logit, i, g, lb, onorm_g):
    Bn, S, H, dk = q.shape
    dv = i.shape[-1]
    lb = lb.reshape(H, dk).astype(jnp.float32)
    log_f = jnp.logaddexp(jnp.log(lb), jnp.log1p(-lb) + jax.nn.log_sigmoid(f_logit.astype(jnp.float32)))
    kk = -jnp.expm1(log_f)
    nc = S // HGRN_CHUNK
    chunk = lambda a: a.astype(jnp.float32).reshape(Bn, nc, HGRN_CHUNK, H, a.shape[-1]).transpose(1, 0, 3, 2, 4)
    qc, kc, vc, lfc = chunk(q), chunk(kk), chunk(i), chunk(log_f)
    bc = jnp.cumsum(lfc, axis=3)
    tril = jnp.arange(HGRN_CHUNK)[:, None] >= jnp.arange(HGRN_CHUNK)[None, :]

    def step(state, xs):
        qj, kj, vj, bj = xs
        diff = bj[:, :, :, None, :] - bj[:, :, None, :, :]
        decay = jnp.exp(jnp.where(tril[:, :, None], diff, -jnp.inf))
        attn = jnp.einsum('bhtd,bhsd,bhtsd->bhts', qj, kj, decay)
        o = jnp.einsum('bhts,bhsv->bhtv', attn, vj) + jnp.einsum('bhtd,bhdv->bhtv', qj * jnp.exp(bj), state)
        b_last = bj[:, :, -1:, :]
        new_state = jnp.exp(b_last[:, :, 0, :])[..., None] * state + jnp.einsum('bhsd,bhsv->bhdv', kj * jnp.exp(b_last - bj), vj)
        return new_state, o

    s0 = jnp.zeros((Bn, H, dk, dv), jnp.float32)
    _, o = lax.scan(step, s0, (qc, kc, vc, bc))
    o = o.transpose(1, 0, 3, 2, 4).reshape(Bn, S, H, dv)
    o = rms_norm(o, onorm_g) * jax.nn.silu(g.astype(jnp.float32))
    return o.reshape(Bn, S, H * dv).astype(q.dtype)


def gmlp_mixer(uv, vn_g, vn_b, ws, bs):
    Bn, S = uv.shape[0], uv.shape[1]
    uv = jax.nn.gelu(uv, approximate=False)
    u, v = uv[..., :C_DIM], uv[..., C_DIM:]
    v = layer_norm(v, vn_g, vn_b)
    nc = S // C_CHUNK
    v = v.reshape(Bn, nc, C_CHUNK, C_GROUPS, C_GROUP_DIM)
    mask = jnp.tril(jnp.ones((C_CHUNK, C_CHUNK), ws.dtype))
    w = ws * mask[None]
    mixed = jnp.einsum('gts,bnsgd->bntgd', w, v) + bs.T[None, None, :, :, None]
    return u * mixed.reshape(Bn, S, C_DIM)


def swiglu(h, w_gate, w_up, w_down):
    return (jax.nn.silu(h @ w_gate) * (h @ w_up)) @ w_down


def moe_swiglu(h, w_router, w_gate, w_up, w_down):
    logits = jnp.einsum('bsd,de->bse', h, w_router).astype(jnp.float32)
    top_v, top_i = lax.top_k(logits, TOP_K)
    wts = jax.nn.softmax(top_v, axis=-1)
    gates = jnp.sum(jax.nn.one_hot(top_i, N_EXPERTS, dtype=jnp.float32) * wts[..., None], axis=-2)

    def per_seq(args):
        hb, gb = args
        a = jnp.einsum('sd,edf->sef', hb, w_gate)
        u = jnp.einsum('sd,edf->sef', hb, w_up)
        act = jax.nn.silu(a) * u * gb[:, :, None]
        return jnp.einsum('sef,efd->sd', act, w_down)

    return lax.map(per_seq, (h, gates.astype(h.dtype)))


def setup_inputs(seed: int = 0) -> dict:
    key = jax.random.key(seed)
    ks = jax.random.split(key, 24)
    nrm = lambda k, shape, s: jax.random.normal(k, shape, jnp.float32) * s
    x = nrm(ks[0], (BATCH, SEQ, D_MODEL), 1.0)
    c = nrm(ks[1], (BATCH, D_MODEL), 1.0)
    offsets = jax.random.randint(ks[2], (BATCH, 1), 0, 4096, dtype=jnp.int32)
    positions = offsets + jnp.arange(SEQ, dtype=jnp.int32)[None, :]
    return {
        "x": x,
        "c": c,
        "positions": positions,
        "w_ada": nrm(ks[3], (DEPTH, D_MODEL, 6 * D_MODEL), 0.5 * D_MODEL ** -0.5),
        "b_ada": nrm(ks[4], (DEPTH, 6 * D_MODEL), 0.02),
        "g_norm_mix": 1.0 + nrm(ks[5], (DEPTH, D_MODEL), 0.02),
        "g_norm_ffn": 1.0 + nrm(ks[6], (DEPTH, D_MODEL), 0.02),
        "w_in": nrm(ks[7], (DEPTH, D_MODEL, IN_DIM), D_MODEL ** -0.5),
        "w_out": nrm(ks[8], (DEPTH, D_MIX, D_MODEL), D_MIX ** -0.5),
        "hgrn_lb_logits": nrm(ks[9], (DEPTH, B_HEADS * B_DK), 0.5),
        "hgrn_out_norm": 1.0 + nrm(ks[10], (DEPTH, B_DV), 0.02),
        "gmlp_vnorm_g": 1.0 + nrm(ks[11], (DEPTH, C_DIM), 0.02),
        "gmlp_vnorm_b": nrm(ks[12], (DEPTH, C_DIM), 0.02),
        "gmlp_w_s": nrm(ks[13], (DEPTH, C_GROUPS, C_CHUNK, C_CHUNK), C_CHUNK ** -0.5),
        "gmlp_b_s": 1.0 + nrm(ks[14], (DEPTH, C_GROUPS, C_CHUNK), 0.02),
        "ffn_w_gate": nrm(ks[15], (N_DENSE, D_MODEL, F_DENSE), D_MODEL ** -0.5),
        "ffn_w_up": nrm(ks[16], (N_DENSE, D_MODEL, F_DENSE), D_MODEL ** -0.5),
        "ffn_w_down": nrm(ks[17], (N_DENSE, F_DENSE, D_MODEL), F_DENSE ** -0.5),
        "moe_w_router": nrm(ks[18], (N_MOE, D_MODEL, N_EXPERTS), D_MODEL ** -0.5),
        "moe_w_gate": nrm(ks[19], (N_MOE, N_EXPERTS, D_MODEL, F_EXPERT), D_MODEL ** -0.5),
        "moe_w_up": nrm(ks[20], (N_MOE, N_EXPERTS, D_MODEL, F_EXPERT), D_MODEL ** -0.5),
        "moe_w_down": nrm(ks[21], (N_MOE, N_EXPERTS, F_EXPERT, D_MODEL), F_EXPERT ** -0.5),
        "g_final": 1.0 + nrm(ks[22], (D_MODEL,), 0.02),
    }


def reference(x, c, positions, w_ada, b_ada, g_norm_mix, g_norm_ffn, w_in, w_out, hgrn_lb_logits, hgrn_out_norm, gmlp_vnorm_g, gmlp_vnorm_b, gmlp_w_s, gmlp_b_s, ffn_w_gate, ffn_w_up, ffn_w_down, moe_w_router, moe_w_gate, moe_w_up, moe_w_down, g_final):
    Bn, S = x.shape[0], x.shape[1]
    p_lb = jax.nn.softmax(hgrn_lb_logits.astype(jnp.float32), axis=0)
    cum = jnp.cumsum(p_lb, axis=0)
    lower_bounds = cum - cum[0:1]
    splits = [int(s) for s in np.cumsum(IN_WIDTHS)[:-1]]
    cond = jax.nn.silu(c)
    for l in range(DEPTH):
        mod = cond @ w_ada[l] + b_ada[l]
        sh1, sc1, g1, sh2, sc2, g2 = [m[:, None, :] for m in jnp.split(mod, 6, axis=-1)]
        h = rms_norm(x, g_norm_mix[l]) * (1 + sc1) + sh1
        z = h @ w_in[l]
        aq, ak, av, iq, ik, iw, bq, bf, bi, bg, cuv = jnp.split(z, splits, axis=-1)
        a_out = dsa_mixer(aq.reshape(Bn, S, A_HEADS, HEAD_DIM), ak, av, iq.reshape(Bn, S, IDX_HEADS, IDX_DIM), ik, iw, positions)
        b_out = hgrn2_mixer(bq.reshape(Bn, S, B_HEADS, B_DK), bf.reshape(Bn, S, B_HEADS, B_DK), bi.reshape(Bn, S, B_HEADS, B_DV), bg.reshape(Bn, S, B_HEADS, B_DV), lower_bounds[l], hgrn_out_norm[l])
        c_out = gmlp_mixer(cuv, gmlp_vnorm_g[l], gmlp_vnorm_b[l], gmlp_w_s[l], gmlp_b_s[l])
        mix = jnp.concatenate([a_out, b_out, c_out], axis=-1) @ w_out[l]
        x = x + g1 * mix
        h = rms_norm(x, g_norm_ffn[l]) * (1 + sc2) + sh2
        if l % 2 == 0:
            y = swiglu(h, ffn_w_gate[l // 2], ffn_w_up[l // 2], ffn_w_down[l // 2])
        else:
            y = moe_swiglu(h, moe_w_router[l // 2], moe_w_gate[l // 2], moe_w_up[l // 2], moe_w_down[l // 2])
        x = x + g2 * y
    return rms_norm(x, g_final)
```

```python
import numpy as np
from contextlib import ExitStack
import concourse.bass as bass
import concourse.mybir as mybir
from concourse.bass_utils import run_bass_kernel_spmd

F32, BF16, I32, U32 = mybir.dt.float32, mybir.dt.bfloat16, mybir.dt.int32, mybir.dt.uint32
AF = mybir.ActivationFunctionType
ALU = mybir.AluOpType
AX = mybir.AxisListType

D = 1024
S = 2048
DEPTH = 2
NCORES = 8
F_DENSE = 2816
F_EXP = 3584
NEXP = 8
EPS = 1e-6
NT = S // 512
NB = S // 128
N_BISECT = 16
TOPK = 256

O_AQ, O_AK, O_AV, O_IQ, O_IK, O_IW = 0, 384, 448, 512, 768, 832
O_BQ, O_BF, O_BI, O_BG, O_CUV = 836, 1220, 1604, 1988, 2372
IN_DIM = 2884


def _swap64(cols):
    cols = np.asarray(cols).reshape(-1, 2, 32)
    return cols[:, ::-1, :].reshape(-1)


def in_col_tiles():
    r = np.arange
    fm = []
    for m in range(3):
        c = r(O_AQ + m * 128, O_AQ + (m + 1) * 128)
        fm.append(c); fm.append(_swap64(c))
    kk = np.concatenate([r(O_AK, O_AK + 64)] * 2)
    fm.append(kk); fm.append(_swap64(kk))
    for m in range(2):
        c = r(O_IQ + m * 128, O_IQ + (m + 1) * 128)
        fm.append(c); fm.append(_swap64(c))
    ik = np.concatenate([r(O_IK, O_IK + 64)] * 2)
    fm.append(ik); fm.append(_swap64(ik))
    for m in range(3):
        fm.append(r(O_BQ + m * 128, O_BQ + (m + 1) * 128))
    for m in range(3):
        fm.append(r(O_BF + m * 128, O_BF + (m + 1) * 128))
    return fm


FM_TILES = in_col_tiles()
T_Q, T_K, T_IQ, T_IK, T_BQ, T_BF = 0, 6, 8, 12, 14, 17
TM_VW = np.concatenate([np.arange(O_AV, O_AV + 64), np.arange(O_IW, O_IW + 4)])
TM_BIG = np.arange(O_BI, O_BI + 768)
TM_CUV = np.arange(O_CUV, O_CUV + 512)


class Eng:
    def __init__(self, name, h, sem, step=1):
        self.name, self.h, self.sem, self.step = name, h, sem, step
        self.count = 0
        self.seen = {}


class Res:
    __slots__ = ("w", "r", "name")

    def __init__(self, name=""):
        self.w = None
        self.r = []
        self.name = name


class K:
    SAME_ENGINE_RAW = True

    def __init__(self, nc, es):
        self.nc, self.es = nc, es
        mk = lambda n: es.enter_context(nc.semaphore(n))
        self.pe = Eng("pe", nc.tensor, mk("s_pe"))
        self.act = Eng("act", nc.scalar, mk("s_act"))
        self.dve = Eng("dve", nc.vector, mk("s_dve"))
        self.pool = Eng("pool", nc.gpsimd, mk("s_pool"))
        self.sp = Eng("sp", nc.sync, mk("s_sp"))
        self.engs = [self.pe, self.act, self.dve, self.pool, self.sp]
        self.slots = []
        self.nsem = 5

    def slot(self, name):
        s = Eng(name, None, self.es.enter_context(self.nc.semaphore(name)), 16)
        self.slots.append(s)
        return s

    def _waits(self, eng, reads, writes):
        need = {}
        for r in reads:
            if r.w is not None:
                e, t = r.w
                need[e] = max(need.get(e, 0), t)
        for w in writes:
            if w.w is not None:
                e, t = w.w
                if e is not eng or e.step == 16:
                    need[e] = max(need.get(e, 0), t)
            for (e, t) in w.r:
                if e is not eng:
                    need[e] = max(need.get(e, 0), t)
        for e, t in need.items():
            if e is eng and (eng is self.pe or not self.SAME_ENGINE_RAW):
                continue
            if eng.seen.get(e, 0) >= t:
                continue
            eng.h.wait_ge(e.sem, t)
            eng.seen[e] = t

    def _done(self, tick, reads, writes):
        for r in reads:
            r.r.append(tick)
        for w in writes:
            w.w = tick
            w.r = []

    def op(self, eng, fn, reads=(), writes=()):
        self._waits(eng, reads, writes)
        inst = fn()
        eng.count += 1
        inst.then_inc(eng.sem, 1)
        self._done((eng, eng.count), reads, writes)

    def dma(self, q, slot, out, in_, reads=(), writes=(), **kw):
        self._waits(q, reads, writes)
        q.h.dma_start(out=out, in_=in_, **kw).then_inc(slot.sem, 16)
        slot.count += 16
        self._done((slot, slot.count), reads, writes)

    def batch(self, slot, res_list):
        for r in res_list:
            r.w = (slot, slot.count)

    def barrier(self):
        allp = self.engs + self.slots
        for e in self.engs:
            for o in allp:
                if o is e or o.count == 0:
                    continue
                if e.seen.get(o, 0) >= o.count:
                    continue
                e.h.wait_ge(o.sem, o.count)
                e.seen[o] = o.count


def vec_layout():
    off = {}
    n = 0

    def add(name, w):
        nonlocal n
        off[name] = (n, w)
        n += w
    add("ctab", 4)
    add("rowm", 4)
    add("hm", 2)
    for l in range(DEPTH):
        add(f"gmix{l}", 8); add(f"gffn{l}", 8); add(f"bada{l}", 48)
        add(f"lbl{l}", 3)
        add(f"bsT{l}", 4)
    add("gfin", 8)
    return off, n


VOFF, NV = vec_layout()


def build(NSEQ, dbg=None, stop=None):
    nc = bass.Bass("TRN2", target_bir_lowering=False)
    dbg = dbg if dbg is not None else {}
    es = ExitStack()
    with es:
        k = K(nc, es)
        PE, ACT, DVE, POOL, SP = k.pe, k.act, k.dve, k.pool, k.sp
        din = lambda n, shp, dt=F32: nc.dram_tensor(n, shp, dt, kind="ExternalInput").ap()
        x_d = din("x", [NSEQ, S, D])
        pos_d = din("pos", [NSEQ, 128, S], I32)
        cT_d = din("cT", [128, 8, NSEQ])
        vecs_d = din("vecs", [128, NV])
        bct_d = din("bct", [DEPTH, 128, 896])
        wada_d = din("w_ada", [DEPTH, D, 6 * D])
        win_d = din("w_in_fm", [DEPTH, len(FM_TILES), 128, 8, 128])
        wvw_d = din("w_in_vw", [DEPTH, 128, 8, 68])
        wbig_d = din("w_in_big", [DEPTH, 128, 8, 768])
        wcuv_d = din("w_in_cuv", [DEPTH, 128, 8, 512])
        wout_d = din("w_out", [DEPTH, D, D])
        wsT_d = din("gmlp_wsT", [DEPTH, 128, 4, 128])
        fg_d = din("ffn_w_gate", [1, D, F_DENSE])
        fu_d = din("ffn_w_up", [1, D, F_DENSE])
        fd_d = din("ffn_w_down", [1, F_DENSE, D])
        wr_d = din("moe_w_router", [1, 128, 8, NEXP])
        mg_d = din("moe_w_gate", [1, NEXP, D, F_EXP])
        mu_d = din("moe_w_up", [1, NEXP, D, F_EXP])
        md_d = din("moe_w_down", [1, NEXP, F_EXP, D])
        cst_d = din("consts", [128, 5, 128])
        y_d = nc.dram_tensor("y", [NSEQ, S, D], F32, kind="ExternalOutput").ap()

        uniq = [0]
        def _nm(n):
            uniq[0] += 1
            return f"sb_{n}_{uniq[0]}"
        sb = lambda n, shp, dt=F32: es.enter_context(nc.sbuf_tensor(_nm(n), shp, dt))
        xT = sb("xT", [128, 8, S]); r_xT = [[Res() for _ in range(NT)] for _ in range(8)]
        r_hT = [Res() for _ in range(NT)]
        r_cat = [[Res() for _ in range(NB)] for _ in range(8)]
        HT = [None]; CT = [None]; XIN = [None]
        vecs = sb("vecs", [128, NV]); r_vecs = Res()
        cst = sb("cst", [128, 5, 128]); r_cst = Res()
        cstb = sb("cstb", [128, 5, 128], BF16); r_cstb = Res()
        modT = sb("modT", [128, DEPTH, 48, NSEQ]); r_mod = Res()
        condT = sb("condT", [128, 8, NSEQ]); r_cond = Res()
        psf = [es.enter_context(nc.psum_tensor(f"psf{i}", [128, 512], F32)) for i in range(6)]
        psb = [es.enter_context(nc.psum_tensor(f"psb{i}", [128, 1024], BF16)) for i in range(2)]
        r_psf = [Res() for _ in range(6)]
        r_psb = [Res() for _ in range(2)]
        sl_c = k.slot("sl_c"); sl_x = k.slot("sl_x"); sl_w = [k.slot(f"sl_w{i}") for i in range(4)]
        sl_o = k.slot("sl_o"); sl_dbg = k.slot("sl_dbg")
        sl_xb = [k.slot(f"sl_xb{i}") for i in range(2)]
        sl_ob = [k.slot(f"sl_ob{i}") for i in range(2)]
        sl_q = [k.slot(f"sl_q{i}") for i in range(2)]
        sl_k = k.slot("sl_k")
        sl_ro = [k.slot(f"sl_ro{i}") for i in range(2)]
        sl_vt = [k.slot(f"sl_vt{i}") for i in range(2)]
        sl_wt = [k.slot(f"sl_wt{i}") for i in range(2)]

        def V(name, j=None):
            o, w = VOFF[name]
            return vecs[:, o:o + w] if j is None else vecs[:, o + j:o + j + 1]

        dumps = []

        def dump(name, ap, res_list, shape, dt):
            if name not in dbg:
                return
            d = nc.dram_tensor("dbg_" + name, shape, dt, kind="ExternalOutput").ap()
            if len(shape) == 3 and shape[2] >= 1024:
                for a in range(shape[1]):
                    k.dma(SP, sl_dbg, d[:, a, :], ap[:, a, :], reads=res_list)
            else:
                k.dma(SP, sl_dbg, d, ap, reads=res_list)
            dumps.append(name)

        k.dma(SP, sl_c, vecs[:], vecs_d[:, :], writes=[r_vecs])
        k.dma(SP, sl_c, cst[:], cst_d[:, :, :], writes=[r_cst])
        k.dma(SP, sl_c, condT[:], cT_d[:, :, :], writes=[r_cond])
        k.batch(sl_c, [r_vecs, r_cst, r_cond])
        k.op(DVE, lambda: nc.vector.tensor_copy(cstb[:], cst[:]), reads=[r_cst], writes=[r_cstb])
        ident = cst[:, 0, :]
        identb = cstb[:, 0, :]
        onesb = cstb[:, 4, :]
        k.op(ACT, lambda: nc.scalar.activation(out=condT[:], in_=condT[:], func=AF.Silu), reads=[r_cond], writes=[r_cond])

        SKIP = dbg.get('_skip', '')
        NST = 2
        wst = [sb(f"wst{i}", [128, 1024]) for i in range(NST)]
        r_wst = [Res() for _ in range(NST)]
        nst = 0
        for l in range(DEPTH if 'mod' not in SKIP else 0):
            for j in range(48):
                st, rs = wst[nst % NST], r_wst[nst % NST]
                stv = st[:].rearrange("p (k c) -> p k c", k=8)
                k.dma(SP, sl_w[nst % NST], stv, wada_d[l].rearrange("(k p) c -> p k c", p=128)[:, :, j * 128:(j + 1) * 128], writes=[rs])
                nst += 1
                bank = psf[j % 2]; rb = r_psf[j % 2]

                def f(stv=stv, bank=bank):
                    for kk_ in range(8):
                        i = nc.tensor.matmul(bank[:, 0:NSEQ], stv[:, kk_, :], condT[:, kk_, :], start=(kk_ == 0), stop=(kk_ == 7))
                    return i
                k.op(PE, f, reads=[rs, r_cond], writes=[rb])
                k.op(DVE, lambda j=j, bank=bank: nc.vector.tensor_scalar(out=modT[:, l, j, :], in0=bank[:, 0:NSEQ], scalar1=V(f"bada{l}", j), scalar2=None, op0=ALU.add),
                     reads=[rb, r_vecs], writes=[r_mod])
        dump("modT", modT[:], [r_mod], [128, DEPTH, 48, NSEQ], F32)

        amod = sb("amod", [128, DEPTH, NSEQ, 2, 8]); r_amod = Res()
        for l in range(DEPTH):
            for s in range(NSEQ):
                for w_, (gn, jo) in enumerate(((f"gmix{l}", 8), (f"gffn{l}", 32))):
                    k.op(DVE, lambda gn=gn, jo=jo, w_=w_, s=s: nc.vector.scalar_tensor_tensor(out=amod[:, l, s, w_, :], in0=modT[:, l, jo:jo + 8, s], scalar=1.0, in1=V(gn), op0=ALU.add, op1=ALU.mult),
                         reads=[r_mod, r_vecs], writes=[r_amod])

        NTMP = 4
        tmp = [sb(f"tmp{i}", [128, 512]) for i in range(NTMP)]
        r_tmp = [Res() for _ in range(NTMP)]
        tctr = [0]

        def T():
            i = tctr[0] % NTMP
            tctr[0] += 1
            return tmp[i], r_tmp[i]
        rsb = [sb(f"rsb{i}", [128, 512]) for i in range(2)]
        r_rsb = [Res() for _ in range(2)]
        rctr = [0]

        def RS():
            i = rctr[0] % 2
            rctr[0] += 1
            return rsb[i], r_rsb[i]
        sqb = [sb(f"sqb{i}", [128, 512], BF16) for i in range(2)]
        r_sqb = [Res() for _ in range(2)]
        r_xin = [Res() for _ in range(2)]
        pctr = [0]

        def PB():
            i = pctr[0] % 6
            pctr[0] += 1
            return psf[i], r_psf[i]

        def load_xT(s):
            for b in range(NB):
                xi, rxi = XIN[0][b % 2], r_xin[b % 2]
                k.dma(SP, sl_xb[b % 2], xi[:], x_d[s, b * 128:(b + 1) * 128, :], writes=[rxi])
                for half in range(2):
                    bank, rb = PB()

                    def f(xi=xi, bank=bank, half=half):
                        for q in range(4):
                            kk_ = half * 4 + q
                            i = nc.tensor.transpose(bank[:, q * 128:(q + 1) * 128], xi[:, kk_ * 128:(kk_ + 1) * 128], ident)
                        return i
                    k.op(PE, f, reads=[rxi, r_cst], writes=[rb])
                    tt = b // 4
                    eng = ACT if half == 0 else DVE
                    if half == 0:
                        k.op(ACT, lambda bank=bank, b=b: nc.scalar.copy(out=xT[:, 0:4, b * 128:(b + 1) * 128], in_=bank[:].rearrange("p (a c) -> p a c", a=4)),
                             reads=[rb], writes=[r_xT[kk_][tt] for kk_ in range(0, 4)])
                    else:
                        k.op(DVE, lambda bank=bank, b=b: nc.vector.tensor_copy(xT[:, 4:8, b * 128:(b + 1) * 128], bank[:].rearrange("p (a c) -> p a c", a=4)),
                             reads=[rb], writes=[r_xT[kk_][tt] for kk_ in range(4, 8)])

        def norm_to_hT(l, s, which, want_f32=None):
            jo_b = 0 if which == 0 else 24
            for t in range(NT):
                ts = slice(t * 512, (t + 1) * 512)
                bank, rb = PB()
                for kk_ in range(8):
                    sq, rsq = sqb[kk_ % 2], r_sqb[kk_ % 2]
                    k.op(ACT, lambda sq=sq, kk_=kk_: nc.scalar.square(sq[:], xT[:, kk_, ts]), reads=[r_xT[kk_][t]], writes=[rsq])
                    k.op(PE, lambda sq=sq, kk_=kk_, bank=bank: nc.tensor.matmul(bank[:], onesb, sq[:], start=(kk_ == 0), stop=(kk_ == 7)), reads=[rsq, r_cstb], writes=[rb])
                if 'n2' in SKIP:
                    continue
                rstd, rr = RS()
                k.op(DVE, lambda rstd=rstd, bank=bank: nc.vector.tensor_scalar(out=rstd[:], in0=bank[:], scalar1=1.0 / D, scalar2=EPS, op0=ALU.mult, op1=ALU.add), reads=[rb], writes=[rr])
                k.op(ACT, lambda rstd=rstd: nc.scalar.activation(out=rstd[:], in_=rstd[:], func=AF.Sqrt), reads=[rr], writes=[rr])
                k.op(DVE, lambda rstd=rstd: nc.vector.reciprocal(rstd[:], rstd[:]), reads=[rr], writes=[rr])
                if 'n1' in SKIP:
                    continue
                for kk_ in range(8):
                    xn, rx = T()
                    k.op(DVE, lambda xn=xn, kk_=kk_, rstd=rstd: nc.vector.tensor_tensor(out=xn[:], in0=xT[:, kk_, ts], in1=rstd[:], op=ALU.mult), reads=[r_xT[kk_][t], rr], writes=[rx])
                    k.op(ACT, lambda xn=xn, kk_=kk_: nc.scalar.activation(out=HT[0][:, kk_, ts], in_=xn[:], func=AF.Identity, scale=amod[:, l, s, which, kk_:kk_ + 1], bias=modT[:, l, jo_b + kk_, s:s + 1]),
                         reads=[rx, r_amod, r_mod], writes=[r_hT[t]])

        def final_out(s):
            for t in range(NT):
                ts = slice(t * 512, (t + 1) * 512)
                bank, rb = PB()
                for kk_ in range(8):
                    sq, rsq = sqb[kk_ % 2], r_sqb[kk_ % 2]
                    k.op(ACT, lambda sq=sq, kk_=kk_: nc.scalar.square(sq[:], xT[:, kk_, ts]), reads=[r_xT[kk_][t]], writes=[rsq])
                    k.op(PE, lambda sq=sq, kk_=kk_, bank=bank: nc.tensor.matmul(bank[:], onesb, sq[:], start=(kk_ == 0), stop=(kk_ == 7)), reads=[rsq, r_cstb], writes=[rb])
                rstd, rr = RS()
                k.op(DVE, lambda rstd=rstd, bank=bank: nc.vector.tensor_scalar(out=rstd[:], in0=bank[:], scalar1=1.0 / D, scalar2=EPS, op0=ALU.mult, op1=ALU.add), reads=[rb], writes=[rr])
                k.op(ACT, lambda rstd=rstd: nc.scalar.activation(out=rstd[:], in_=rstd[:], func=AF.Sqrt), reads=[rr], writes=[rr])
                k.op(DVE, lambda rstd=rstd: nc.vector.reciprocal(rstd[:], rstd[:]), reads=[rr], writes=[rr])
                for kk_ in range(8):
                    k.op(DVE, lambda kk_=kk_, rstd=rstd: nc.vector.scalar_tensor_tensor(out=xT[:, kk_, ts], in0=xT[:, kk_, ts], scalar=V("gfin", kk_), in1=rstd[:], op0=ALU.mult, op1=ALU.mult),
                         reads=[r_xT[kk_][t], rr, r_vecs], writes=[r_xT[kk_][t]])
                for bb in range(4):
                    b = t * 4 + bb
                    xi, rxi = XIN[0][b % 2], r_xin[b % 2]
                    for half in range(2):
                        bank2, rb2 = PB()

                        def f(bank2=bank2, half=half, b=b):
                            for q in range(4):
                                kk_ = half * 4 + q
                                i = nc.tensor.transpose(bank2[:, q * 128:(q + 1) * 128], xT[:, kk_, b * 128:(b + 1) * 128], ident)
                            return i
                        k.op(PE, f, reads=[r_xT[kk_][t] for kk_ in range(half * 4, half * 4 + 4)] + [r_cst], writes=[rb2])
                        if half == 0:
                            k.op(ACT, lambda bank2=bank2, xi=xi: nc.scalar.copy(out=xi[:, 0:512], in_=bank2[:]), reads=[rb2], writes=[rxi])
                        else:
                            k.op(DVE, lambda bank2=bank2, xi=xi: nc.vector.tensor_copy(xi[:, 512:1024], bank2[:]), reads=[rb2], writes=[rxi])
                    k.dma(SP, sl_ob[b % 2], y_d[s, b * 128:(b + 1) * 128, :], xi[:], reads=[rxi])

        wctr = [0]

        def load_cast(dst_ap, src_ap, r_dst, shape, eng=None):
            a, b_ = shape[1], shape[2]
            STG = 1024
            if b_ > STG:
                for b0 in range(0, b_, STG):
                    bw = min(STG, b_ - b0)
                    load_cast(dst_ap[:, :, b0:b0 + bw], src_ap[:, :, b0:b0 + bw], r_dst, [128, a, bw], eng)
                return
            ach = max(1, STG // b_)
            for a0 in range(0, a, ach):
                aw = min(ach, a - a0)
                i = wctr[0] % NST
                wctr[0] += 1
                st, rs = wst[i], r_wst[i]
                sv = st[:, 0:aw * b_].rearrange("p (a b) -> p a b", a=aw)
                k.dma(SP, sl_w[i], sv, src_ap[:, a0:a0 + aw, :], writes=[rs])
                if eng is ACT:
                    k.op(ACT, lambda sv=sv, a0=a0, aw=aw: nc.scalar.copy(out=dst_ap[:, a0:a0 + aw, :], in_=sv), reads=[rs], writes=[r_dst])
                else:
                    k.op(POOL, lambda sv=sv, a0=a0, aw=aw: nc.gpsimd.tensor_copy(dst_ap[:, a0:a0 + aw, :], sv), reads=[rs], writes=[r_dst])

        def mixer_phase(l, s):
            es2 = ExitStack()
            with es2:
                sb2 = lambda n, shp, dt=F32: es2.enter_context(nc.sbuf_tensor(_nm(n), shp, dt))
                wfm = []
                r_wfm = [Res() for _ in range(4)]
                fctr = [0]

                def fm_weights(tile_idx):
                    if not wfm:
                        wfm.extend(sb2(f"wfm{i}", [128, 8, 128], BF16) for i in range(4))
                    i = fctr[0] % 4
                    fctr[0] += 1
                    load_cast(wfm[i][:], win_d[l, tile_idx], r_wfm[i], [128, 8, 128])
                    return wfm[i], r_wfm[i]

                def fm_proj(w, rw, t, bank, rb):
                    def f():
                        for kk_ in range(8):
                            i = nc.tensor.matmul(bank[:], w[:, kk_, :], HT[0][:, kk_, t * 512:(t + 1) * 512], start=(kk_ == 0), stop=(kk_ == 7))
                        return i
                    k.op(PE, f, reads=[rw, r_hT[t]], writes=[rb])

                if "c" in PH:
                    with Scope() as a3:
                        gmlp(l, s, a3)
                if "b" in PH:
                    with Scope() as a3:
                        hgrn(l, s, a3, fm_weights, fm_proj)
                if "a" in PH:
                    with Scope() as a3:
                        dsa_proj(l, s, a3, fm_weights, fm_proj)
            k.barrier()

        def to_catT(tok_ap, r_tok, nchunks, chunk0, b):
            bank, rb = psb[0], r_psb[0]

            def f():
                for c in range(nchunks):
                    i = nc.tensor.transpose(bank[:, c * 128:(c + 1) * 128], tok_ap[:, c * 128:(c + 1) * 128], identb)
                return i
            k.op(PE, f, reads=[r_tok, r_cstb], writes=[rb])
            k.op(ACT, lambda: nc.scalar.copy(out=CT[0][:, chunk0:chunk0 + nchunks, b * 128:(b + 1) * 128], in_=bank[:, 0:nchunks * 128].rearrange("p (a c) -> p a c", a=nchunks)),
                 reads=[rb], writes=[r_cat[chunk0 + c][b] for c in range(nchunks)])

        def gmlp(l, s, sb2):
            wc = sb2("wc", [128, 8, 512], BF16); r_wc = Res()
            for kh in range(2):
                load_cast(wc[:, kh * 4:(kh + 1) * 4, :], wcuv_d[l, :, kh * 4:(kh + 1) * 4, :], r_wc, [128, 4, 512])
            wsT = sb2("wsT", [128, 4, 128], BF16); r_ws = Res()
            wsf = sb2("wsf", [128, 4, 128]); r_wsf = Res()
            k.dma(SP, sl_c, wsf[:], wsT_d[l], writes=[r_wsf])
            bcl = sb2("bcl", [128, 512]); r_bcl = Res()
            k.dma(SP, sl_c, bcl[:], bct_d[l, :, 384:896], writes=[r_bcl])
            k.batch(sl_c, [r_wsf, r_bcl])
            for g in range(4):
                k.op(DVE, lambda g=g: nc.vector.tensor_tensor(out=wsT[:, g, :], in0=wsf[:, g, :], in1=cst[:, 1, :], op=ALU.mult), reads=[r_wsf, r_cst], writes=[r_ws])
            uv = [sb2(f"uv{i}", [128, 512]) for i in range(2)]; r_uv = [Res() for _ in range(2)]
            vn = [sb2(f"vn{i}", [128, 256], BF16) for i in range(2)]; r_vn = [Res() for _ in range(2)]
            ctok = [sb2(f"ctok{i}", [128, 256], BF16) for i in range(2)]; r_ct = [Res() for _ in range(2)]
            st6 = sb2("st6", [128, 2, 8]); r_st = Res()
            for b in range(NB):
                t = b // 4
                bank, rb = PB()

                def f(bank=bank, b=b):
                    for kk_ in range(8):
                        i = nc.tensor.matmul(bank[:], HT[0][:, kk_, b * 128:(b + 1) * 128], wc[:, kk_, :], start=(kk_ == 0), stop=(kk_ == 7))
                    return i
                k.op(PE, f, reads=[r_wc, r_hT[t]], writes=[rb])
                u, ru = uv[b % 2], r_uv[b % 2]
                k.op(ACT, lambda u=u, bank=bank: nc.scalar.activation(out=u[:], in_=bank[:], func=AF.Gelu), reads=[rb], writes=[ru])
                stt = st6[:, b % 2, :]
                k.op(DVE, lambda u=u, stt=stt: nc.vector.bn_stats(out=stt[:, 0:6], in_=u[:, 256:512]), reads=[ru], writes=[r_st])
                k.op(DVE, lambda stt=stt: nc.vector.bn_aggr(out=stt[:, 6:8], in_=stt[:, 0:6]), reads=[r_st], writes=[r_st])
                k.op(DVE, lambda stt=stt: nc.vector.tensor_scalar(out=stt[:, 7:8], in0=stt[:, 7:8], scalar1=EPS, scalar2=None, op0=ALU.add), reads=[r_st], writes=[r_st])
                k.op(ACT, lambda stt=stt: nc.scalar.activation(out=stt[:, 7:8], in_=stt[:, 7:8], func=AF.Sqrt), reads=[r_st], writes=[r_st])
                k.op(DVE, lambda stt=stt: nc.vector.reciprocal(stt[:, 7:8], stt[:, 7:8]), reads=[r_st], writes=[r_st])
                k.op(DVE, lambda u=u, stt=stt: nc.vector.tensor_scalar(out=u[:, 256:512], in0=u[:, 256:512], scalar1=stt[:, 6:7], scalar2=stt[:, 7:8], op0=ALU.subtract, op1=ALU.mult), reads=[ru, r_st], writes=[ru])
                k.op(POOL, lambda u=u: nc.gpsimd.tensor_tensor(out=u[:, 256:512], in0=u[:, 256:512], in1=bcl[:, 0:256], op=ALU.mult), reads=[ru, r_bcl], writes=[ru])
                v_, rv = vn[b % 2], r_vn[b % 2]
                k.op(POOL, lambda u=u, v_=v_: nc.gpsimd.tensor_tensor(out=v_[:], in0=u[:, 256:512], in1=bcl[:, 256:512], op=ALU.add), reads=[ru, r_bcl], writes=[rv])
                bank2, rb2 = PB()

                def f2(bank2=bank2, v_=v_):
                    for g in range(4):
                        i = nc.tensor.matmul(bank2[:, g * 64:(g + 1) * 64], wsT[:, g, :], v_[:, g * 64:(g + 1) * 64], start=True, stop=True)
                    return i
                k.op(PE, f2, reads=[r_ws, rv], writes=[rb2])
                ct, rc = ctok[b % 2], r_ct[b % 2]
                for g in range(4):
                    k.op(DVE, lambda g=g, bank2=bank2, u=u, ct=ct: nc.vector.scalar_tensor_tensor(out=ct[:, g * 64:(g + 1) * 64], in0=bank2[:, g * 64:(g + 1) * 64], scalar=V(f"bsT{l}", g), in1=u[:, g * 64:(g + 1) * 64], op0=ALU.add, op1=ALU.mult),
                         reads=[rb2, ru, r_vecs], writes=[rc])
                to_catT(ct[:], rc, 2, 6, b)

        def hgrn(l, s, sb2, fm_weights, fm_proj):
            wbq = [sb2(f"wbq{m}", [128, 8, 128], BF16) for m in range(2)]; r_wbq = [Res() for _ in range(2)]
            wbf = [sb2(f"wbf{m}", [128, 8, 128], BF16) for m in range(2)]; r_wbf = [Res() for _ in range(2)]
            wbig = sb2("wbig", [128, 8, 768], BF16); r_wbig = Res()
            for kh in range(4):
                load_cast(wbig[:, kh * 2:(kh + 1) * 2, :], wbig_d[l, :, kh * 2:(kh + 1) * 2, :], r_wbig, [128, 2, 768])
            lbv = sb2("lbv", [128, 8, 3]); r_lb = Res()
            L0, L1 = V("lbl0"), V("lbl1")
            if l == 0:
                k.op(DVE, lambda: nc.vector.memset(lbv[:, 0, :], 0.0), writes=[r_lb])
            else:
                k.op(DVE, lambda: nc.vector.tensor_tensor(out=lbv[:, 1, :], in0=L0, in1=L1, op=ALU.max), reads=[r_vecs], writes=[r_lb])
                k.op(DVE, lambda: nc.vector.tensor_tensor(out=lbv[:, 2, :], in0=L0, in1=lbv[:, 1, :], op=ALU.subtract), reads=[r_vecs, r_lb], writes=[r_lb])
                k.op(DVE, lambda: nc.vector.tensor_tensor(out=lbv[:, 3, :], in0=L1, in1=lbv[:, 1, :], op=ALU.subtract), reads=[r_vecs, r_lb], writes=[r_lb])
                k.op(ACT, lambda: nc.scalar.activation(out=lbv[:, 2:4, :], in_=lbv[:, 2:4, :], func=AF.Exp), reads=[r_lb], writes=[r_lb])
                k.op(DVE, lambda: nc.vector.tensor_tensor(out=lbv[:, 1, :], in0=lbv[:, 2, :], in1=lbv[:, 3, :], op=ALU.add), reads=[r_lb], writes=[r_lb])
                k.op(DVE, lambda: nc.vector.reciprocal(lbv[:, 1, :], lbv[:, 1, :]), reads=[r_lb], writes=[r_lb])
                k.op(DVE, lambda: nc.vector.tensor_tensor(out=lbv[:, 0, :], in0=lbv[:, 3, :], in1=lbv[:, 1, :], op=ALU.mult), reads=[r_lb], writes=[r_lb])
            k.op(DVE, lambda: nc.vector.tensor_scalar(out=lbv[:, 4, :], in0=lbv[:, 0, :], scalar1=-1.0, scalar2=1.0, op0=ALU.mult, op1=ALU.add), reads=[r_lb], writes=[r_lb])
            k.op(DVE, lambda: nc.vector.tensor_scalar(out=lbv[:, 5, :], in0=lbv[:, 4, :], scalar1=-1.0, scalar2=None, op0=ALU.mult), reads=[r_lb], writes=[r_lb])
            onl = sb2("onl", [128, 384]); r_onl = Res()
            k.dma(SP, sl_c, onl[:], bct_d[l, :, 0:384], writes=[r_onl])
            rst = sb2("rst", [128, 512], BF16); r_rst = Res()
            k.op(DVE, lambda: nc.vector.memset(rst[:], 1.0), writes=[r_rst])
            k.op(DVE, lambda: nc.vector.memset(rst[:].rearrange("p (c j) -> p c j", j=32)[:, :, 0:1], 0.0), writes=[r_rst])
            ft = {"kk": tmp[0], "sig": tmp[1], "logf": tmp[1], "eb": tmp[1], "bcum": tmp[2], "enb": tmp[2]}
            rft = {"kk": r_tmp[0], "sig": r_tmp[1], "logf": r_tmp[1], "eb": r_tmp[1], "bcum": r_tmp[2], "enb": r_tmp[2]}
            qp = sb2("qp", [128, 3, 512], BF16); r_qp = Res()
            kppz = sb2("kppz", [128, 2, 3, 512], BF16); r_kpp = Res()
            k3 = sb2("k3", [128, 512], BF16); r_k3 = Res()
            Dm = sb2("Dm", [128, 3, 16]); r_Dm = Res()
            ktok = sb2("ktok", [128, 4, 384], BF16); r_ktok = Res()
            kz = sb2("kz", [128, 4, 384], BF16); r_kz = Res()
            vtok = sb2("vtok", [128, 384], BF16); r_vtok = Res()
            gsg = tmp[0][:, 0:384]; r_gsg = r_tmp[0]
            St = sb2("St", [128, 3, 128]); r_S = Res()
            Sbf = sb2("Sbf", [128, 4, 3, 128], BF16); r_Sbf = Res()
            attnT = sb2("attnT", [128, 6, 128], BF16); r_at = Res()
            Zq = sb2("Zq", [128, 3, 4, 128], BF16); r_Zq = Res()
            osq = tmp[3][:, 0:384]; r_osq = r_tmp[3]
            ss = sb2("ss", [128, 8]); r_ss = Res()
            btok = sb2("btok", [128, 384], BF16); r_bt = Res()
            k.op(DVE, lambda: nc.vector.memset(St[:], 0.0), writes=[r_S])
            k.op(POOL, lambda: nc.gpsimd.memset(Zq[:], 0.0), writes=[r_Zq])
            k.op(POOL, lambda: nc.gpsimd.memset(Sbf[:], 0.0), writes=[r_Sbf])
            HS = int(dbg.get('_hs', 99))
            if HS <= 1:
                return
            for t in range(NT):
                ts = slice(t * 512, (t + 1) * 512)
                for m in range(3):
                    bq, rbq = PB()
                    bf_, rbf = PB()
                    wi = (t * 3 + m) % 2
                    load_cast(wbq[wi][:], win_d[l, T_BQ + m], r_wbq[wi], [128, 8, 128])
                    load_cast(wbf[wi][:], win_d[l, T_BF + m], r_wbf[wi], [128, 8, 128])
                    fm_proj(wbq[wi], r_wbq[wi], t, bq, rbq)
                    fm_proj(wbf[wi], r_wbf[wi], t, bf_, rbf)
                    k.op(ACT, lambda bf_=bf_: nc.scalar.activation(out=ft["sig"][:], in_=bf_[:], func=AF.Sigmoid), reads=[rbf], writes=[rft["sig"]])
                    k.op(DVE, lambda m=m: nc.vector.tensor_scalar(out=ft["kk"][:], in0=ft["sig"][:], scalar1=lbv[:, 5, m:m + 1], scalar2=lbv[:, 4, m:m + 1], op0=ALU.mult, op1=ALU.add), reads=[rft["sig"], r_lb], writes=[rft["kk"]])
                    k.op(DVE, lambda m=m: nc.vector.tensor_scalar(out=ft["logf"][:], in0=ft["sig"][:], scalar1=lbv[:, 4, m:m + 1], scalar2=lbv[:, 0, m:m + 1], op0=ALU.mult, op1=ALU.add), reads=[rft["sig"], r_lb], writes=[rft["logf"]])
                    k.op(ACT, lambda: nc.scalar.activation(out=ft["logf"][:], in_=ft["logf"][:], func=AF.Ln), reads=[rft["logf"]], writes=[rft["logf"]])
                    k.op(DVE, lambda: nc.vector.tensor_tensor_scan(out=ft["bcum"][:], data0=rst[:], data1=ft["logf"][:], initial=0.0, op0=ALU.mult, op1=ALU.add), reads=[r_rst, rft["logf"]], writes=[rft["bcum"]])
                    if HS <= 2:
                        continue
                    k.op(ACT, lambda: nc.scalar.activation(out=ft["eb"][:], in_=ft["bcum"][:], func=AF.Exp), reads=[rft["bcum"]], writes=[rft["eb"]])
                    k.op(ACT, lambda: nc.scalar.activation(out=ft["enb"][:], in_=ft["bcum"][:], func=AF.Exp, scale=-1.0), reads=[rft["bcum"]], writes=[rft["enb"]])
                    k.op(DVE, lambda m=m, bq=bq: nc.vector.tensor_tensor(out=qp[:, m, :], in0=bq[:], in1=ft["eb"][:], op=ALU.mult), reads=[rbq, rft["eb"]], writes=[r_qp])
                    k.op(POOL, lambda m=m: nc.gpsimd.tensor_tensor(out=ft["kk"][:], in0=ft["kk"][:], in1=ft["enb"][:], op=ALU.mult), reads=[rft["kk"], rft["enb"]], writes=[rft["kk"]])
                    for hp in range(2):
                        k.op(POOL, lambda m=m, hp=hp: nc.gpsimd.tensor_scalar(out=kppz[:, hp, m, :], in0=ft["kk"][:], scalar1=V("hm", hp), scalar2=None, op0=ALU.mult), reads=[rft["kk"], r_vecs], writes=[r_kpp])
                    k.op(POOL, lambda m=m: nc.gpsimd.tensor_copy(Dm[:, m, :], ft["eb"][:].rearrange("p (c j) -> p c j", j=32)[:, :, 31]), reads=[rft["eb"]], writes=[r_Dm])
                    k.op(POOL, lambda m=m: nc.gpsimd.tensor_tensor(out=k3[:].rearrange("p (c j) -> p c j", j=32), in0=ft["kk"][:].rearrange("p (c j) -> p c j", j=32), in1=Dm[:, m, :].to_broadcast([128, 16, 32]), op=ALU.mult),
                         reads=[rft["kk"], r_Dm], writes=[r_k3])
                    if HS <= 3:
                        continue
                    bank, rb = psb[1], r_psb[1]

                    def f(bank=bank):
                        for bb in range(4):
                            i = nc.tensor.transpose(bank[:, bb * 128:(bb + 1) * 128], k3[:, bb * 128:(bb + 1) * 128], identb)
                        return i
                    k.op(PE, f, reads=[r_k3, r_cstb], writes=[rb])
                    k.op(ACT, lambda bank=bank, m=m: nc.scalar.copy(out=ktok[:, :, m * 128:(m + 1) * 128], in_=bank[:, 0:512].rearrange("p (a c) -> p a c", a=4)), reads=[rb], writes=[r_ktok])
                for bb in range(4 if HS > 4 else 0):
                    b = t * 4 + bb
                    bs_ = slice(b * 128, (b + 1) * 128)
                    ls_ = slice(bb * 128, (bb + 1) * 128)
                    b1, rb1 = PB()
                    b2, rb2 = PB()

                    def f(b1=b1, b2=b2, bs_=bs_):
                        for kk_ in range(8):
                            nc.tensor.matmul(b1[:, 0:384], HT[0][:, kk_, bs_], wbig[:, kk_, 0:384], start=(kk_ == 0), stop=(kk_ == 7))
                        for kk_ in range(8):
                            i = nc.tensor.matmul(b2[:, 0:384], HT[0][:, kk_, bs_], wbig[:, kk_, 384:768], start=(kk_ == 0), stop=(kk_ == 7))
                        return i
                    k.op(PE, f, reads=[r_wbig, r_hT[t]], writes=[rb1, rb2])
                    k.op(ACT, lambda b1=b1: nc.scalar.copy(out=vtok[:], in_=b1[:, 0:384]), reads=[rb1], writes=[r_vtok])
                    k.op(ACT, lambda b2=b2: nc.scalar.activation(out=gsg, in_=b2[:, 0:384], func=AF.Silu), reads=[rb2], writes=[r_gsg])
                    k.op(POOL, lambda: nc.gpsimd.tensor_tensor(out=gsg, in0=gsg, in1=onl[:], op=ALU.mult), reads=[r_gsg, r_onl], writes=[r_gsg])
                    for c in range(4):
                        k.op(POOL, lambda c=c, bb=bb: nc.gpsimd.tensor_scalar(out=kz[:, c, :], in0=ktok[:, bb, :], scalar1=V("rowm", c), scalar2=None, op0=ALU.mult), reads=[r_ktok, r_vecs], writes=[r_kz])
                    for m in range(3):
                        for c in range(4):
                            k.op(POOL, lambda c=c, m=m, bb=bb: nc.gpsimd.tensor_copy(Zq[:, m, c, c * 32:(c + 1) * 32], qp[:, m, bb * 128 + c * 32:bb * 128 + (c + 1) * 32]), reads=[r_qp], writes=[r_Zq])
                    if HS <= 5:
                        continue
                    for ch in range(2):
                        bU = []
                        for c2 in range(2):
                            c = ch * 2 + c2
                            bu_, rbu_ = PB()
                            bU.append((bu_, rbu_))

                            def f(bu_=bu_, c=c):
                                for m in range(3):
                                    i = nc.tensor.matmul(bu_[:, m * 128:(m + 1) * 128], kz[:, c, m * 128:(m + 1) * 128], vtok[:, m * 128:(m + 1) * 128], start=True, stop=True)
                                return i
                            k.op(PE, f, reads=[r_kz, r_vtok], writes=[rbu_])
                        for c2 in range(2):
                            c = ch * 2 + c2
                            cc = bb * 4 + c
                            bu_, rbu_ = bU[c2]
                            for hp in range(2):
                                k.op(ACT, lambda c=c, hp=hp: nc.scalar.copy(out=Sbf[64 * hp:64 * hp + 64, c, :, 64 * hp:64 * hp + 64], in_=St[64 * hp:64 * hp + 64, :, 64 * hp:64 * hp + 64]), reads=[r_S], writes=[r_Sbf])
                            for m in range(3):
                                k.op(DVE, lambda m=m, cc=cc, bu_=bu_: nc.vector.scalar_tensor_tensor(out=St[:, m, :], in0=St[:, m, :], scalar=Dm[:, m, cc:cc + 1], in1=bu_[:, m * 128:(m + 1) * 128], op0=ALU.mult, op1=ALU.add),
                                     reads=[r_S, r_Dm, rbu_], writes=[r_S])
                    if HS <= 6:
                        continue
                    bA, rbA = PB()
                    bB, rbB = PB()

                    def f(bA=bA, bB=bB, ls_=ls_):
                        for h in range(6):
                            m, hp = h // 2, h % 2
                            bank = bA if h < 4 else bB
                            if 'hp0' in SKIP and hp == 1:
                                continue
                            i = nc.tensor.matmul(bank[:, (h % 4) * 128:(h % 4 + 1) * 128], kppz[:, hp, m, ls_], qp[:, m, ls_], start=True, stop=True)
                        return i
                    k.op(PE, f, reads=[r_kpp, r_qp], writes=[rbA, rbB])
                    for h in range(6):
                        bank, rbk = (bA, rbA) if h < 4 else (bB, rbB)
                        k.op(DVE, lambda h=h, bank=bank: nc.vector.tensor_tensor(out=attnT[:, h, :], in0=bank[:, (h % 4) * 128:(h % 4 + 1) * 128], in1=cst[:, 3, :], op=ALU.mult), reads=[rbk, r_cst], writes=[r_at])
                    if HS <= 7:
                        continue
                    bO, rbO = PB()

                    def f(bO=bO):
                        for h in range(6):
                            m, hp = h // 2, h % 2
                            nc.tensor.matmul(bO[:, h * 64:(h + 1) * 64], attnT[:, h, :], vtok[:, h * 64:(h + 1) * 64], start=True, stop=False)
                            for c in range(4):
                                i = nc.tensor.matmul(bO[:, h * 64:(h + 1) * 64], Zq[:, m, c, :], Sbf[:, c, m, 64 * hp:64 * hp + 64], start=False, stop=(c == 3))
                        return i
                    k.op(PE, f, reads=[r_at, r_vtok, r_Zq, r_Sbf], writes=[rbO])
                    k.op(ACT, lambda bO=bO: nc.scalar.square(osq, bO[:, 0:384]), reads=[rbO], writes=[r_osq])
                    k.op(DVE, lambda: nc.vector.tensor_reduce(out=ss[:, 0:6], in_=osq.rearrange("p (h d) -> p h d", d=64), axis=AX.X, op=ALU.add), reads=[r_osq], writes=[r_ss])
                    k.op(DVE, lambda: nc.vector.tensor_scalar(out=ss[:, 0:6], in0=ss[:, 0:6], scalar1=1.0 / 64, scalar2=EPS, op0=ALU.mult, op1=ALU.add), reads=[r_ss], writes=[r_ss])
                    k.op(ACT, lambda: nc.scalar.activation(out=ss[:, 0:6], in_=ss[:, 0:6], func=AF.Sqrt), reads=[r_ss], writes=[r_ss])
                    k.op(DVE, lambda: nc.vector.reciprocal(ss[:, 0:6], ss[:, 0:6]), reads=[r_ss], writes=[r_ss])
                    k.op(DVE, lambda bO=bO: nc.vector.tensor_tensor(out=osq.rearrange("p (h d) -> p h d", d=64), in0=bO[:, 0:384].rearrange("p (h d) -> p h d", d=64), in1=ss[:, 0:6].to_broadcast([128, 6, 64]), op=ALU.mult),
                         reads=[rbO, r_ss, r_osq], writes=[r_osq])
                    k.op(POOL, lambda: nc.gpsimd.tensor_tensor(out=btok[:], in0=osq, in1=gsg, op=ALU.mult), reads=[r_osq, r_gsg], writes=[r_bt])
                    to_catT(btok[:], r_bt, 3, 3, b)

        dq = nc.dram_tensor("scr_q", [7, 128, S], BF16, kind="Internal").ap()
        dv = nc.dram_tensor("scr_v", [S, 64], BF16, kind="Internal").ap()
        dw = nc.dram_tensor("scr_w", [S, 4], F32, kind="Internal").ap()
        r_dq = Res(); r_dv = Res(); r_dw = Res()
        sl_s = k.slot("sl_s")
        PI = float(np.pi)

        def dsa_proj(l, s, sb2, fm_weights, fm_proj):
            wfl = [sb2(f"wfl{i}", [128, 8, 128], BF16) for i in range(4)]
            r_wfl = [Res() for _ in range(4)]
            fcl = [0]

            def fm_weights(tile_idx):
                i = fcl[0] % 4
                fcl[0] += 1
                load_cast(wfl[i][:], win_d[l, tile_idx], r_wfl[i], [128, 8, 128])
                return wfl[i], r_wfl[i]
            posi = sb2("posi", [128, 512], I32); r_posi = Res()
            ang = sb2("ang", [128, 512]); r_ang = Res()
            nf = sb2("nf", [128, 512]); r_nf = Res()
            ni = sb2("ni", [128, 512], I32); r_ni = Res()
            cs = sb2("cs", [128, 2, 512]); r_cs = Res()
            ro = [sb2(f"ro{i}", [128, 512], BF16) for i in range(2)]; r_ro = [Res() for _ in range(2)]
            wvw = sb2("wvw", [128, 8, 68], BF16); r_wvw = Res()
            load_cast(wvw[:], wvw_d[l], r_wvw, [128, 8, 68])
            vt = [sb2(f"vt{i}", [128, 64], BF16) for i in range(2)]; r_vt = [Res() for _ in range(2)]
            wt = [sb2(f"wt{i}", [128, 4]) for i in range(2)]; r_wt = [Res() for _ in range(2)]

            def wrap(dst, r_dst):
                k.op(DVE, lambda: nc.vector.tensor_scalar(out=nf[:], in0=dst, scalar1=PI, scalar2=-2.0 * PI, op0=ALU.is_gt, op1=ALU.mult), reads=[r_dst], writes=[r_nf])
                k.op(DVE, lambda: nc.vector.tensor_tensor(out=dst, in0=dst, in1=nf[:], op=ALU.add), reads=[r_dst, r_nf], writes=[r_dst])
                k.op(DVE, lambda: nc.vector.tensor_scalar(out=nf[:], in0=dst, scalar1=-PI, scalar2=2.0 * PI, op0=ALU.is_lt, op1=ALU.mult), reads=[r_dst], writes=[r_nf])
                k.op(DVE, lambda: nc.vector.tensor_tensor(out=dst, in0=dst, in1=nf[:], op=ALU.add), reads=[r_dst, r_nf], writes=[r_dst])
            cnt = 0
            for t in range(NT):
                ts = slice(t * 512, (t + 1) * 512)
                k.dma(SP, sl_c, posi[:], pos_d[s, :, ts], writes=[r_posi])
                k.op(DVE, lambda: nc.vector.tensor_copy(ang[:], posi[:]), reads=[r_posi], writes=[r_ang])
                k.op(DVE, lambda: nc.vector.tensor_scalar(out=ang[:], in0=ang[:], scalar1=V("ctab", 0), scalar2=None, op0=ALU.mult), reads=[r_ang, r_vecs], writes=[r_ang])
                k.op(DVE, lambda: nc.vector.tensor_scalar(out=ni[:], in0=ang[:], scalar1=1.0 / (2.0 * PI), scalar2=None, op0=ALU.mult), reads=[r_ang], writes=[r_ni])
                k.op(DVE, lambda: nc.vector.tensor_copy(nf[:], ni[:]), reads=[r_ni], writes=[r_nf])
                k.op(DVE, lambda: nc.vector.scalar_tensor_tensor(out=cs[:, 1, :], in0=nf[:], scalar=-2.0 * PI, in1=ang[:], op0=ALU.mult, op1=ALU.add), reads=[r_nf, r_ang], writes=[r_cs])
                wrap(cs[:, 1, :], r_cs)
                k.op(DVE, lambda: nc.vector.tensor_scalar(out=cs[:, 0, :], in0=cs[:, 1, :], scalar1=PI / 2, scalar2=None, op0=ALU.add), reads=[r_cs], writes=[r_cs])
                wrap(cs[:, 0, :], r_cs)
                k.op(ACT, lambda: nc.scalar.activation(out=cs[:], in_=cs[:], func=AF.Sin), reads=[r_cs], writes=[r_cs])
                k.op(DVE, lambda: nc.vector.tensor_scalar(out=cs[:, 1, :], in0=cs[:, 1, :], scalar1=V("ctab", 1), scalar2=None, op0=ALU.mult), reads=[r_cs, r_vecs], writes=[r_cs])
                for fam, ti in enumerate((0, 2, 4, 6, 8, 10, 12)):
                    w0, rw0 = fm_weights(ti)
                    w1, rw1 = fm_weights(ti + 1)
                    bp, rbp = PB()
                    bs2, rbs2 = PB()
                    fm_proj(w0, rw0, t, bp, rbp)
                    fm_proj(w1, rw1, t, bs2, rbs2)
                    t1, rt1 = T()
                    t2, rt2 = T()
                    k.op(DVE, lambda bp=bp, t1=t1: nc.vector.tensor_tensor(out=t1[:], in0=bp[:], in1=cs[:, 0, :], op=ALU.mult), reads=[rbp, r_cs], writes=[rt1])
                    k.op(DVE, lambda bs2=bs2, t2=t2: nc.vector.tensor_tensor(out=t2[:], in0=bs2[:], in1=cs[:, 1, :], op=ALU.mult), reads=[rbs2, r_cs], writes=[rt2])
                    o_, ro_ = ro[cnt % 2], r_ro[cnt % 2]
                    sro = sl_ro[cnt % 2]
                    cnt += 1
                    k.op(POOL, lambda t1=t1, t2=t2, o_=o_: nc.gpsimd.tensor_tensor(out=o_[:], in0=t1[:], in1=t2[:], op=ALU.add), reads=[rt1, rt2], writes=[ro_])
                    k.dma(SP, sro, dq[fam, :, ts], o_[:], reads=[ro_], writes=[r_dq])
                for bb in range(4):
                    b = t * 4 + bb
                    bk, rbk = PB()

                    def f(bk=bk, b=b):
                        for kk_ in range(8):
                            i = nc.tensor.matmul(bk[:, 0:68], HT[0][:, kk_, b * 128:(b + 1) * 128], wvw[:, kk_, :], start=(kk_ == 0), stop=(kk_ == 7))
                        return i
                    k.op(PE, f, reads=[r_wvw, r_hT[t]], writes=[rbk])
                    v_, rv_ = vt[b % 2], r_vt[b % 2]
                    w_, rw_ = wt[b % 2], r_wt[b % 2]
                    k.op(ACT, lambda bk=bk, v_=v_: nc.scalar.copy(out=v_[:], in_=bk[:, 0:64]), reads=[rbk], writes=[rv_])
                    k.op(DVE, lambda bk=bk, w_=w_: nc.vector.tensor_copy(w_[:], bk[:, 64:68]), reads=[rbk], writes=[rw_])
                    k.dma(SP, sl_vt[b % 2], dv[b * 128:(b + 1) * 128, :], v_[:], reads=[rv_], writes=[r_dv])
                    k.dma(SP, sl_wt[b % 2], dw[b * 128:(b + 1) * 128, :], w_[:], reads=[rw_], writes=[r_dw])

        def dsa_core(l, s, sb2):
            kz = sb2("kTz", [128, 2, S], BF16); r_kz = Res()
            ikz = sb2("ikTz", [128, 2, S], BF16); r_ikz = Res()
            k.op(POOL, lambda: nc.gpsimd.memset(kz[:], 0.0), writes=[r_kz])
            k.op(POOL, lambda: nc.gpsimd.memset(ikz[:], 0.0), writes=[r_ikz])
            for hp in range(2):
                k.dma(SP, sl_k, kz[64 * hp:64 * hp + 64, hp, :], dq[3, 64 * hp:64 * hp + 64, :], reads=[r_dq], writes=[r_kz])
                k.dma(SP, sl_k, ikz[64 * hp:64 * hp + 64, hp, :], dq[6, 64 * hp:64 * hp + 64, :], reads=[r_dq], writes=[r_ikz])
            va = sb2("va", [128, NB, 65], BF16); r_va = Res()
            k.op(POOL, lambda: nc.gpsimd.memset(va[:], 1.0), writes=[r_va])
            k.dma(SP, sl_k, va[:, :, 0:64], dv.rearrange("(b p) d -> p b d", p=128), reads=[r_dv], writes=[r_va])
            iwa = sb2("iwa", [128, NB, 4]); r_iw = Res()
            k.dma(SP, sl_k, iwa[:], dw.rearrange("(b p) d -> p b d", p=128), reads=[r_dw], writes=[r_iw])
            k.batch(sl_k, [r_kz, r_ikz, r_va, r_iw])
            qb = [sb2(f"qb{i}", [128, 5, 128], BF16) for i in range(2)]; r_qb = [Res() for _ in range(2)]
            sc = sb2("sc", [128, S]); r_sc = Res()
            junk = sb2("junk", [128, S], BF16); r_junk = Res()
            msk = sb2("msk", [128, S], BF16); r_msk = Res()
            mT = sb2("mT", [128, NB, 128], BF16); r_mT = Res()
            ex = [sb2(f"ex{i}", [128, 512], BF16) for i in range(3)]; r_ex = [Res() for _ in range(3)]
            pT = [sb2(f"pT{i}", [128, 512], BF16) for i in range(3)]; r_pT = [Res() for _ in range(3)]
            rl = [sb2(f"rl{i}", [128, 512]) for i in range(2)]; r_rl = [Res() for _ in range(2)]
            bis = sb2("bis", [128, 8 + N_BISECT]); r_bis = Res()
            p2 = sb2("p2", [128, N_BISECT]); r_p2 = Res()
            for i in range(N_BISECT):
                k.op(DVE, lambda i=i: nc.vector.memset(p2[:, i:i + 1], 0.5 ** (i + 1)), writes=[r_p2])
            atok = sb2("atok", [128, 384], BF16); r_atok = Res()
            rec = sb2("rec", [128, 8]); r_rec = Res()
            LO, HI, MID, CNT, GS = 0, 1, 2, 3, 4
            ectr = 0
            ninf = sb2("ninf", [128, 128]); r_ninf = Res()
            k.op(DVE, lambda: nc.vector.tensor_scalar(out=ninf[:], in0=cst[:, 2, :], scalar1=-1.0, scalar2=1e30, op0=ALU.add, op1=ALU.mult), reads=[r_cst], writes=[r_ninf])
            def S1(j):
                n = (j + 1) * 128
                q_, rq_ = qb[j % 2], r_qb[j % 2]
                for i5, fam in enumerate((0, 1, 2, 4, 5)):
                    k.dma(SP, sl_q[j % 2], q_[:, i5, :], dq[fam, :, j * 128:(j + 1) * 128], reads=[r_dq], writes=[rq_])
                if j >= 2:
                    for c0 in range(0, n, 512):
                        cw = min(512, n - c0)
                        for h in range(4):
                            bk, rbk = PB()
                            k.op(PE, lambda bk=bk, h=h, c0=c0, cw=cw: nc.tensor.matmul(bk[:, 0:cw], q_[:, 3 + h // 2, :], ikz[:, h % 2, c0:c0 + cw], start=True, stop=True), reads=[rq_, r_ikz], writes=[rbk])
                            r_, rr_ = rl[h % 2], r_rl[h % 2]
                            k.op(ACT, lambda bk=bk, r_=r_, cw=cw: nc.scalar.activation(out=r_[:, 0:cw], in_=bk[:, 0:cw], func=AF.Relu), reads=[rbk], writes=[rr_])
                            if h == 0:
                                k.op(DVE, lambda r_=r_, c0=c0, cw=cw, h=h: nc.vector.tensor_scalar(out=sc[:, c0:c0 + cw], in0=r_[:, 0:cw], scalar1=iwa[:, j, h:h + 1], scalar2=None, op0=ALU.mult), reads=[rr_, r_iw], writes=[r_sc])
                            else:
                                k.op(DVE, lambda r_=r_, c0=c0, cw=cw, h=h: nc.vector.scalar_tensor_tensor(out=sc[:, c0:c0 + cw], in0=r_[:, 0:cw], scalar=iwa[:, j, h:h + 1], in1=sc[:, c0:c0 + cw], op0=ALU.mult, op1=ALU.add), reads=[rr_, r_iw, r_sc], writes=[r_sc])
                    dg = slice(j * 128, (j + 1) * 128)
                    k.op(DVE, lambda: nc.vector.tensor_tensor(out=sc[:, dg], in0=sc[:, dg], in1=cst[:, 2, :], op=ALU.mult), reads=[r_sc, r_cst], writes=[r_sc])
                    k.op(DVE, lambda: nc.vector.tensor_tensor(out=sc[:, dg], in0=sc[:, dg], in1=ninf[:], op=ALU.add), reads=[r_sc, r_ninf], writes=[r_sc])
                    k.op(DVE, lambda: nc.vector.tensor_reduce(out=bis[:, HI:HI + 1], in_=sc[:, 0:n], axis=AX.X, op=ALU.max), reads=[r_sc], writes=[r_bis])
                    k.op(DVE, lambda: nc.vector.tensor_reduce(out=bis[:, LO:LO + 1], in_=sc[:, 0:n - 128], axis=AX.X, op=ALU.min), reads=[r_sc], writes=[r_bis])
                    k.op(DVE, lambda: nc.vector.tensor_tensor(out=bis[:, GS:GS + 1], in0=bis[:, HI:HI + 1], in1=bis[:, LO:LO + 1], op=ALU.subtract), reads=[r_bis], writes=[r_bis])
                    k.op(DVE, lambda: nc.vector.tensor_scalar(out=bis[:, 8:8 + N_BISECT], in0=p2[:], scalar1=bis[:, GS:GS + 1], scalar2=None, op0=ALU.mult), reads=[r_bis, r_p2], writes=[r_bis])
                    for it in range(N_BISECT):
                        k.op(DVE, lambda it=it: nc.vector.tensor_tensor(out=bis[:, MID:MID + 1], in0=bis[:, LO:LO + 1], in1=bis[:, 8 + it:9 + it], op=ALU.add), reads=[r_bis], writes=[r_bis])
                        k.op(DVE, lambda: nc.vector.tensor_scalar(out=junk[:, 0:n], in0=sc[:, 0:n], scalar1=bis[:, MID:MID + 1], scalar2=0.0, op0=ALU.is_ge, op1=ALU.add, accum_out=bis[:, CNT:CNT + 1]), reads=[r_sc, r_bis], writes=[r_junk, r_bis])
                        k.op(DVE, lambda it=it: nc.vector.tensor_scalar(out=bis[:, GS:GS + 1], in0=bis[:, CNT:CNT + 1], scalar1=float(TOPK), scalar2=bis[:, 8 + it:9 + it], op0=ALU.is_ge, op1=ALU.mult), reads=[r_bis], writes=[r_bis])
                        k.op(DVE, lambda: nc.vector.tensor_tensor(out=bis[:, LO:LO + 1], in0=bis[:, LO:LO + 1], in1=bis[:, GS:GS + 1], op=ALU.add), reads=[r_bis], writes=[r_bis])
                    k.op(DVE, lambda: nc.vector.tensor_scalar(out=msk[:, 0:n], in0=sc[:, 0:n], scalar1=bis[:, LO:LO + 1], scalar2=None, op0=ALU.is_ge), reads=[r_sc, r_bis], writes=[r_msk])
                else:
                    if j == 1:
                        k.op(DVE, lambda: nc.vector.memset(msk[:, 0:128], 1.0), writes=[r_msk])
                    k.op(DVE, lambda: nc.vector.tensor_copy(msk[:, j * 128:(j + 1) * 128], cst[:, 2, :]), reads=[r_cst], writes=[r_msk])

            def S2(j):
                for g0 in range(0, j + 1, 8):
                    gn = min(8, j + 1 - g0)
                    bank, rb = psb[1], r_psb[1]

                    def f(bank=bank, g0=g0, gn=gn):
                        for g in range(gn):
                            i = nc.tensor.transpose(bank[:, g * 128:(g + 1) * 128], msk[:, (g0 + g) * 128:(g0 + g + 1) * 128], identb)
                        return i
                    k.op(PE, f, reads=[r_msk, r_cstb], writes=[rb])
                    k.op(ACT, lambda bank=bank, g0=g0, gn=gn: nc.scalar.copy(out=mT[:, g0:g0 + gn, :], in_=bank[:, 0:gn * 128].rearrange("p (a c) -> p a c", a=gn)), reads=[rb], writes=[r_mT])

            def S3(j):
                nonlocal ectr
                q_, rq_ = qb[j % 2], r_qb[j % 2]
                bO, rbO = PB()
                for h in range(6):
                    m, hp = h // 2, h % 2
                    for g0 in range(0, j + 1, 4):
                        gn = min(4, j + 1 - g0)
                        bl, rbl = PB()
                        if bl is bO:
                            bl, rbl = PB()

                        def f(bl=bl, g0=g0, gn=gn, m=m, hp=hp):
                            for g in range(gn):
                                i = nc.tensor.matmul(bl[:, g * 128:(g + 1) * 128], kz[:, hp, (g0 + g) * 128:(g0 + g + 1) * 128], q_[:, m, :], start=True, stop=True)
                            return i
                        k.op(PE, f, reads=[r_kz, rq_], writes=[rbl])
                        e_, re_ = ex[ectr % 3], r_ex[ectr % 3]
                        p_, rp_ = pT[ectr % 3], r_pT[ectr % 3]
                        ectr += 1
                        k.op(ACT, lambda bl=bl, e_=e_, gn=gn: nc.scalar.activation(out=e_[:, 0:gn * 128], in_=bl[:, 0:gn * 128], func=AF.Exp, scale=0.125), reads=[rbl], writes=[re_])
                        k.op(POOL, lambda e_=e_, p_=p_, g0=g0, gn=gn: nc.gpsimd.tensor_tensor(out=p_[:, 0:gn * 128], in0=e_[:, 0:gn * 128], in1=mT[:, g0:g0 + gn, :].rearrange("p a c -> p (a c)"), op=ALU.mult), reads=[re_, r_mT], writes=[rp_])

                        def f2(p_=p_, g0=g0, gn=gn, h=h):
                            for g in range(gn):
                                i = nc.tensor.matmul(bO[:, h * 65:(h + 1) * 65], p_[:, g * 128:(g + 1) * 128], va[:, g0 + g, :], start=(g0 + g == 0), stop=(g0 + g == j))
                            return i
                        k.op(PE, f2, reads=[rp_, r_va], writes=[rbO])
                k.op(DVE, lambda: nc.vector.reciprocal(rec[:, 0:6], bO[:, 0:390].rearrange("p (h d) -> p h d", d=65)[:, :, 64]), reads=[rbO], writes=[r_rec])
                k.op(DVE, lambda: nc.vector.tensor_tensor(out=atok[:].rearrange("p (h d) -> p h d", d=64), in0=bO[:, 0:390].rearrange("p (h d) -> p h d", d=65)[:, :, 0:64], in1=rec[:, 0:6].to_broadcast([128, 6, 64]), op=ALU.mult), reads=[rbO, r_rec], writes=[r_atok])
                to_catT(atok[:], r_atok, 3, 0, j)

            J0 = int(dbg.get('_j0', 0))
            NJ = min(NB, int(dbg.get('_nj', NB)))
            S1(J0)
            S2(J0)
            for j in range(J0, NJ):
                if j + 1 < NJ:
                    S1(j + 1)
                S3(j)
                if j + 1 < NJ:
                    S2(j + 1)

        def out_proj(l, s):
            es2 = ExitStack()
            with es2:
                wo = es2.enter_context(nc.sbuf_tensor(_nm("wo"), [128, 8, 1024], BF16)); r_wo = Res()
                for kk_ in range(8):
                    for hh in range(2):
                        load_cast(wo[:, kk_:kk_ + 1, hh * 512:(hh + 1) * 512], wout_d[l, kk_ * 128:(kk_ + 1) * 128, hh * 512:(hh + 1) * 512].rearrange("p (a c) -> p a c", a=1), r_wo, [128, 1, 512])
                for t in range(NT):
                    for d in range(8):
                        bank, rb = PB()

                        def f(bank=bank, d=d, t=t):
                            for kk_ in range(8):
                                i = nc.tensor.matmul(bank[:], wo[:, kk_, d * 128:(d + 1) * 128], CT[0][:, kk_, t * 512:(t + 1) * 512], start=(kk_ == 0), stop=(kk_ == 7))
                            return i
                        k.op(PE, f, reads=[r_wo] + [r_cat[kk_][t * 4 + bb] for kk_ in range(8) for bb in range(4)], writes=[rb])
                        k.op(DVE, lambda bank=bank, d=d, t=t: nc.vector.scalar_tensor_tensor(out=xT[:, d, t * 512:(t + 1) * 512], in0=bank[:], scalar=modT[:, l, 16 + d, s:s + 1], in1=xT[:, d, t * 512:(t + 1) * 512], op0=ALU.mult, op1=ALU.add),
                             reads=[rb, r_mod, r_xT[d][t]], writes=[r_xT[d][t]])
            k.barrier()

        def ffn_phase(l, s):
            moe = (l % 2 == 1)
            es2 = ExitStack()
            with es2:
                sb2 = lambda n, shp, dt=F32: es2.enter_context(nc.sbuf_tensor(_nm(n), shp, dt))
                F = F_EXP if moe else F_DENSE
                wg = [sb2(f"wg{i}", [128, 8, 512], BF16) for i in range(2)]; r_wg = [Res() for _ in range(2)]
                wu = [sb2(f"wu{i}", [128, 8, 512], BF16) for i in range(2)]; r_wu = [Res() for _ in range(2)]
                wd = [sb2(f"wd{i}", [128, 4, 1024], BF16) for i in range(2)]; r_wd = [Res() for _ in range(2)]
                actT = [sb2(f"actT{i}", [128, 4, 512], BF16) for i in range(2)]; r_act = [Res() for _ in range(2)]
                sil = [sb2(f"sil{i}", [128, 512]) for i in range(2)]; r_sil = [Res() for _ in range(2)]
                gbc = None
                if moe:
                    gbc = sb2("gbc", [128, S], BF16); r_gbc = Res()
                    gt = sb2("gt", [128, NB, NEXP]); r_gt = Res()
                    gsel = [sb2(f"gsel{i}", [128, 128]) for i in range(2)]; r_gsel = [Res() for _ in range(2)]
                    moe_gates(l, s, sb2, gt, r_gt)
                fgs = [(f0, min(512, F - f0)) for f0 in range(0, F, 512)]
                groups = [(e, f0, fw) for e in range(NEXP if moe else 1) for (f0, fw) in fgs]
                units = [(gi, t) for gi in range(len(groups)) for t in range(NT)]

                def load_group(gi):
                    e, f0, fw = groups[gi]
                    nf = fw // 128
                    i2 = gi % 2
                    Wg = mg_d[0, e] if moe else fg_d[0]
                    Wu = mu_d[0, e] if moe else fu_d[0]
                    Wd = md_d[0, e] if moe else fd_d[0]
                    for kh in range(2):
                        load_cast(wg[i2][:, kh * 4:(kh + 1) * 4, 0:fw], Wg.rearrange("(k p) f -> p k f", p=128)[:, kh * 4:(kh + 1) * 4, f0:f0 + fw], r_wg[i2], [128, 4, fw])
                        load_cast(wu[i2][:, kh * 4:(kh + 1) * 4, 0:fw], Wu.rearrange("(k p) f -> p k f", p=128)[:, kh * 4:(kh + 1) * 4, f0:f0 + fw], r_wu[i2], [128, 4, fw])
                    for fh in range(0, nf, 2):
                        n2 = min(2, nf - fh)
                        load_cast(wd[i2][:, fh:fh + n2, :], Wd.rearrange("(k p) d -> p k d", p=128)[:, f0 // 128 + fh:f0 // 128 + fh + n2, :], r_wd[i2], [128, n2, 1024])

                def gu(ui):
                    gi, t = units[ui]
                    e, f0, fw = groups[gi]
                    nf = fw // 128
                    i2 = gi % 2
                    ts = slice(t * 512, (t + 1) * 512)
                    if moe and t == 0 and f0 == 0:
                        make_gbc(e, gt, r_gt, gbc, r_gbc, gsel, r_gsel)
                    a_, ra = actT[ui % 2], r_act[ui % 2]
                    for fi in range(nf):
                        bg, rbg = PB()
                        bu, rbu = PB()

                        def f(bg=bg, bu=bu, fi=fi):
                            for kk_ in range(8):
                                nc.tensor.matmul(bg[:], wg[i2][:, kk_, fi * 128:(fi + 1) * 128], HT[0][:, kk_, ts], start=(kk_ == 0), stop=(kk_ == 7))
                            for kk_ in range(8):
                                i = nc.tensor.matmul(bu[:], wu[i2][:, kk_, fi * 128:(fi + 1) * 128], HT[0][:, kk_, ts], start=(kk_ == 0), stop=(kk_ == 7))
                            return i
                        k.op(PE, f, reads=[r_wg[i2], r_wu[i2], r_hT[t]], writes=[rbg, rbu])
                        sl_, rs_ = sil[fi % 2], r_sil[fi % 2]
                        k.op(ACT, lambda sl_=sl_, bg=bg: nc.scalar.activation(out=sl_[:], in_=bg[:], func=AF.Silu), reads=[rbg], writes=[rs_])
                        if moe:
                            k.op(DVE, lambda sl_=sl_, bu=bu: nc.vector.tensor_tensor(out=sl_[:], in0=sl_[:], in1=bu[:], op=ALU.mult), reads=[rs_, rbu], writes=[rs_])
                            k.op(DVE, lambda sl_=sl_, fi=fi: nc.vector.tensor_tensor(out=a_[:, fi, :], in0=sl_[:], in1=gbc[:, ts], op=ALU.mult),
                                 reads=[rs_, r_gbc], writes=[ra])
                        else:
                            k.op(DVE, lambda sl_=sl_, bu=bu, fi=fi: nc.vector.tensor_tensor(out=a_[:, fi, :], in0=sl_[:], in1=bu[:], op=ALU.mult), reads=[rs_, rbu], writes=[ra])

                def yy(ui):
                    gi, t = units[ui]
                    e, f0, fw = groups[gi]
                    nf = fw // 128
                    i2 = gi % 2
                    ts = slice(t * 512, (t + 1) * 512)
                    a_, ra = actT[ui % 2], r_act[ui % 2]
                    for d in range(8):
                        by, rby = PB()

                        def f(by=by, d=d):
                            for fi in range(nf):
                                i = nc.tensor.matmul(by[:], wd[i2][:, fi, d * 128:(d + 1) * 128], a_[:, fi, :], start=(fi == 0), stop=(fi == nf - 1))
                            return i
                        k.op(PE, f, reads=[r_wd[i2], ra], writes=[rby])
                        k.op(DVE, lambda by=by, d=d: nc.vector.scalar_tensor_tensor(out=xT[:, d, ts], in0=by[:], scalar=modT[:, l, 40 + d, s:s + 1], in1=xT[:, d, ts], op0=ALU.mult, op1=ALU.add),
                             reads=[rby, r_mod, r_xT[d][t]], writes=[r_xT[d][t]])

                load_group(0)
                gu(0)
                for ui in range(len(units)):
                    gi, t = units[ui]
                    if t == 0 and gi + 1 < len(groups):
                        load_group(gi + 1)
                    if ui + 1 < len(units):
                        gu(ui + 1)
                    yy(ui)
            k.barrier()

        def moe_gates(l, s, sb2, gt, r_gt):
            wrf = sb2("wrf", [128, 8, NEXP]); r_wrf = Res()
            wrb = sb2("wrb", [128, 8, NEXP], BF16); r_wrb = Res()
            k.dma(SP, sl_c, wrf[:], wr_d[0], writes=[r_wrf])
            k.op(DVE, lambda: nc.vector.tensor_copy(wrb[:], wrf[:]), reads=[r_wrf], writes=[r_wrb])
            lg = sb2("lg", [128, 4, NEXP]); r_lg = Res()
            m_ = sb2("mx", [128, 4]); r_m = Res()
            for b in range(NB):
                bk, rbk = PB()

                def f(bk=bk, b=b):
                    for kk_ in range(8):
                        i = nc.tensor.matmul(bk[:, 0:NEXP], HT[0][:, kk_, b * 128:(b + 1) * 128], wrb[:, kk_, :], start=(kk_ == 0), stop=(kk_ == 7))
                    return i
                k.op(PE, f, reads=[r_wrb, r_hT[b // 4]], writes=[rbk])
                k.op(DVE, lambda bk=bk: nc.vector.tensor_copy(lg[:, 0, :], bk[:, 0:NEXP]), reads=[rbk], writes=[r_lg])
                k.op(DVE, lambda: nc.vector.tensor_reduce(out=m_[:, 0:1], in_=lg[:, 0, :], axis=AX.X, op=ALU.max), reads=[r_lg], writes=[r_m])
                k.op(DVE, lambda: nc.vector.tensor_scalar(out=lg[:, 1, :], in0=lg[:, 0, :], scalar1=m_[:, 0:1], scalar2=-1e30, op0=ALU.is_equal, op1=ALU.mult), reads=[r_lg, r_m], writes=[r_lg])
                k.op(DVE, lambda: nc.vector.tensor_tensor(out=lg[:, 1, :], in0=lg[:, 1, :], in1=lg[:, 0, :], op=ALU.add), reads=[r_lg], writes=[r_lg])
                k.op(DVE, lambda: nc.vector.tensor_reduce(out=m_[:, 1:2], in_=lg[:, 1, :], axis=AX.X, op=ALU.max), reads=[r_lg], writes=[r_m])
                k.op(DVE, lambda: nc.vector.tensor_scalar(out=lg[:, 2, :], in0=lg[:, 0, :], scalar1=m_[:, 1:2], scalar2=None, op0=ALU.is_ge), reads=[r_lg, r_m], writes=[r_lg])
                k.op(DVE, lambda: nc.vector.tensor_scalar(out=m_[:, 2:3], in0=m_[:, 0:1], scalar1=-1.0, scalar2=None, op0=ALU.mult), reads=[r_m], writes=[r_m])
                k.op(ACT, lambda: nc.scalar.activation(out=lg[:, 3, :], in_=lg[:, 0, :], func=AF.Exp, bias=m_[:, 2:3], scale=1.0), reads=[r_lg, r_m], writes=[r_lg])
                k.op(DVE, lambda: nc.vector.tensor_tensor(out=lg[:, 3, :], in0=lg[:, 3, :], in1=lg[:, 2, :], op=ALU.mult), reads=[r_lg], writes=[r_lg])
                k.op(DVE, lambda: nc.vector.tensor_reduce(out=m_[:, 3:4], in_=lg[:, 3, :], axis=AX.X, op=ALU.add), reads=[r_lg], writes=[r_m])
                k.op(DVE, lambda: nc.vector.reciprocal(m_[:, 3:4], m_[:, 3:4]), reads=[r_m], writes=[r_m])
                k.op(DVE, lambda b=b: nc.vector.tensor_scalar(out=gt[:, b, :], in0=lg[:, 3, :], scalar1=m_[:, 3:4], scalar2=None, op0=ALU.mult), reads=[r_lg, r_m], writes=[r_gt])

        def make_gbc(e, gt, r_gt, gbc, r_gbc, gsel, r_gsel):
            for t in range(NT):
                bk, rbk = PB()
                for bb in range(4):
                    b = t * 4 + bb
                    g_, rg_ = gsel[b % 2], r_gsel[b % 2]
                    k.op(DVE, lambda g_=g_, b=b: nc.vector.tensor_scalar(out=g_[:], in0=cst[:, 4, :], scalar1=gt[:, b, e:e + 1], scalar2=None, op0=ALU.mult), reads=[r_cst, r_gt], writes=[rg_])
                    k.op(PE, lambda g_=g_, bk=bk, bb=bb: nc.tensor.matmul(bk[:, bb * 128:(bb + 1) * 128], g_[:], ident, start=True, stop=True), reads=[rg_, r_cst], writes=[rbk])
                k.op(ACT, lambda bk=bk, t=t: nc.scalar.copy(out=gbc[:, t * 512:(t + 1) * 512], in_=bk[:]), reads=[rbk], writes=[r_gbc])

        PH = dbg.get("_phases", "abc")

        class Scope:
            def __init__(self):
                self.es = ExitStack()

            def __enter__(self):
                self.es.__enter__()
                return lambda n, shp, dt=F32: self.es.enter_context(nc.sbuf_tensor(_nm(n), shp, dt))

            def __exit__(self, *a):
                k.barrier()
                return self.es.__exit__(*a)

        def run_seq(s):
            with Scope() as a:
                XIN[0] = [a(f"xin{i}", [128, 1024]) for i in range(2)]
                load_xT(s)
            if stop == "load":
                dump("xT", xT[:], [r for rr in r_xT for r in rr], [128, 8, S], F32)
                return False
            for l in range(DEPTH):
                with Scope() as a:
                    CT[0] = a("catT", [128, 8, S], BF16)
                    with Scope() as a2:
                        HT[0] = a2("hT", [128, 8, S], BF16)
                        norm_to_hT(l, s, 0)
                        if stop == f"norm{l}":
                            dump("hT", HT[0][:], r_hT, [128, 8, S], BF16)
                            return False
                        mixer_phase(l, s)
                    if "a" in PH and 'dcore' not in SKIP:
                        with Scope() as a3:
                            dsa_core(l, s, a3)
                    if stop == f"mix{l}":
                        dump("catT", CT[0][:], [r for rr in r_cat for r in rr], [128, 8, S], BF16)
                        return False
                    out_proj(l, s)
                if stop == f"outp{l}":
                    dump("xT", xT[:], [r for rr in r_xT for r in rr], [128, 8, S], F32)
                    return False
                with Scope() as a:
                    HT[0] = a("h2T", [128, 8, S], BF16)
                    norm_to_hT(l, s, 1)
                    ffn_phase(l, s)
                if stop == f"ffn{l}":
                    dump("xT", xT[:], [r for rr in r_xT for r in rr], [128, 8, S], F32)
                    return False
            with Scope() as a:
                XIN[0] = [a(f"xin{i}", [128, 1024]) for i in range(2)]
                final_out(s)
            return True

        for s in range(NSEQ if 'all' not in SKIP else 0):
            if not run_seq(s):
                break
        k.barrier()
        if dbg.get('_verbose'):
            print('COUNTS', {e.name: e.count for e in k.engs + k.slots}, flush=True)
    return nc


def _consts():
    c = np.zeros((128, 5, 128), np.float32)
    i = np.arange(128)
    c[:, 0, :] = np.eye(128, dtype=np.float32)
    c[:, 1, :] = (i[:, None] <= i[None, :])
    c[:, 2, :] = (i[None, :] <= i[:, None])
    c[:, 3, :] = (i[:, None] <= i[None, :]) & ((i[:, None] // 32) == (i[None, :] // 32))
    c[:, 4, :] = 1.0
    return c


def prep_shared(inp):
    f = lambda a: np.ascontiguousarray(a, dtype=np.float32)
    vec = np.zeros((128, NV), np.float32)

    def put(name, arr):
        o, w = VOFF[name]
        vec[:, o:o + w] = arr
    p = np.arange(128)
    ctab = np.zeros((128, 4), np.float32)
    ctab[:, 0] = (10000.0 ** (-np.arange(0, 64, 2, dtype=np.float32) / 64))[p % 32]
    ctab[:, 1] = np.where((p % 64) < 32, -1.0, 1.0)
    put("ctab", ctab)
    put("rowm", (p[:, None] // 32 == np.arange(4)[None, :]).astype(np.float32))
    put("hm", (p[:, None] // 64 == np.arange(2)[None, :]).astype(np.float32))
    for l in range(DEPTH):
        put(f"gmix{l}", inp["g_norm_mix"][l].reshape(8, 128).T)
        put(f"gffn{l}", inp["g_norm_ffn"][l].reshape(8, 128).T)
        put(f"bada{l}", inp["b_ada"][l].reshape(48, 128).T)
        put(f"lbl{l}", inp["hgrn_lb_logits"][l].reshape(3, 128).T)
        put(f"bsT{l}", inp["gmlp_b_s"][l].T)
    put("gfin", inp["g_final"].reshape(8, 128).T)
    w_in = inp["w_in"]
    tile_pk = lambda w: f(w.reshape(8, 128, w.shape[-1]).transpose(1, 0, 2))
    bct = np.zeros((DEPTH, 128, 896), np.float32)
    for l in range(DEPTH):
        bct[l, :, 0:384] = np.tile(inp["hgrn_out_norm"][l], 6)[None, :]
        bct[l, :, 384:640] = inp["gmlp_vnorm_g"][l][None, :]
        bct[l, :, 640:896] = inp["gmlp_vnorm_b"][l][None, :]
    sh = {
        "vecs": vec, "bct": bct,
        "w_ada": f(inp["w_ada"]),
        "w_in_fm": f(np.stack([np.stack([tile_pk(w_in[l][:, c]) for c in FM_TILES]) for l in range(DEPTH)])),
        "w_in_vw": f(np.stack([tile_pk(w_in[l][:, TM_VW]) for l in range(DEPTH)])),
        "w_in_big": f(np.stack([tile_pk(w_in[l][:, TM_BIG]) for l in range(DEPTH)])),
        "w_in_cuv": f(np.stack([tile_pk(w_in[l][:, TM_CUV]) for l in range(DEPTH)])),
        "w_out": f(inp["w_out"]),
        "gmlp_wsT": f(inp["gmlp_w_s"].transpose(0, 3, 1, 2)),
        "ffn_w_gate": f(inp["ffn_w_gate"]), "ffn_w_up": f(inp["ffn_w_up"]), "ffn_w_down": f(inp["ffn_w_down"]),
        "moe_w_router": f(inp["moe_w_router"].reshape(1, 8, 128, NEXP).transpose(0, 2, 1, 3)),
        "moe_w_gate": f(inp["moe_w_gate"]), "moe_w_up": f(inp["moe_w_up"]), "moe_w_down": f(inp["moe_w_down"]),
        "consts": _consts(),
    }
    return sh


def prep_core(inp, seqs):
    n = len(seqs)
    x = np.ascontiguousarray(inp["x"][seqs], dtype=np.float32)
    pos = np.ascontiguousarray(np.broadcast_to(inp["positions"][seqs][:, None, :], (n, 128, S)).astype(np.int32))
    cT = np.ascontiguousarray(inp["c"][seqs].reshape(n, 8, 128).transpose(2, 1, 0), dtype=np.float32)
    return {"x": x, "pos": pos, "cT": cT}


_NC_CACHE = {}


def kernel(**inputs):
    inp = {k_: np.asarray(v) for k_, v in inputs.items()}
    B = inp["x"].shape[0]
    nseq = B // NCORES
    if nseq not in _NC_CACHE:
        _NC_CACHE[nseq] = build(nseq)
    nc = _NC_CACHE[nseq]
    sh = prep_shared(inp)
    in_maps = []
    for c in range(NCORES):
        m = dict(sh)
        m.update(prep_core(inp, list(range(c * nseq, (c + 1) * nseq))))
        in_maps.append(m)
    res = run_bass_kernel_spmd(nc, in_maps, core_ids=list(range(NCORES)))
    return np.concatenate([r["y"] for r in res.results], axis=0).astype(np.float32)
```
